# Optimizing a Trainium2 kernel written in Bass

```python
import math
import jax
import jax.numpy as jnp
from jax import lax
import numpy as np

D_MODEL = 2048
BATCH = 4
SEQ = 2048
DEPTH = 2

HEAD_DIM = 128
N_MIX_HEADS = D_MODEL // HEAD_DIM
NSA_HEADS = N_MIX_HEADS // 4
SWA_HEADS = N_MIX_HEADS // 4
GDN_HEADS = N_MIX_HEADS - NSA_HEADS - SWA_HEADS
SWA_KV_HEADS = max(1, SWA_HEADS // 2)
N_SOFTMAX_HEADS = NSA_HEADS + SWA_HEADS
NSA_WIDTH = NSA_HEADS * HEAD_DIM
GDN_WIDTH = GDN_HEADS * HEAD_DIM
SWA_WIDTH = SWA_HEADS * HEAD_DIM
SWA_KV_WIDTH = SWA_KV_HEADS * HEAD_DIM
MIX_WIDTH = NSA_WIDTH + GDN_WIDTH + SWA_WIDTH

CMP_BLOCK = 32
CMP_STRIDE = 16
CMP_HIDDEN = 2 * HEAD_DIM
SEL_BLOCK = 64
SEL_TOPN = 8
NSA_WINDOW = 512
FORCE_BONUS = 1000.0

GDN_CONV = 4
GDN_CHUNK = 64

SWA_WINDOW = 128
Q_BLOCK = 128

D_FF = 7168
N_EXPERTS = 8
TOP_K = 2
EXPERT_FF = 7168
MOE_BLOCK = 512
N_DENSE = (DEPTH + 1) // 2
N_MOE = DEPTH // 2

NORM_EPS = 1e-6
NEG_INF = -1e30

IN_SPLIT_SIZES = (NSA_WIDTH, HEAD_DIM, HEAD_DIM, HEAD_DIM, HEAD_DIM, HEAD_DIM, HEAD_DIM, 3 * NSA_HEADS,
                  GDN_WIDTH, GDN_WIDTH, GDN_WIDTH, GDN_WIDTH, GDN_HEADS, GDN_HEADS,
                  SWA_WIDTH, SWA_KV_WIDTH, SWA_KV_WIDTH)
D_IN = sum(IN_SPLIT_SIZES)

kernel_name = "hybrid_nsa_gdn_swa_moe_trunk"


def rmsnorm(x, w):
    xf = x.astype(jnp.float32)
    y = xf * lax.rsqrt(jnp.mean(xf * xf, axis=-1, keepdims=True) + NORM_EPS)
    return (y * w.astype(jnp.float32)).astype(x.dtype)


def l2norm(t):
    return t * lax.rsqrt(jnp.sum(t * t, axis=-1, keepdims=True) + NORM_EPS)


def masked_softmax(logits, mask):
    return jax.nn.softmax(jnp.where(mask, logits, NEG_INF), axis=-1)


def alibi_slopes():
    i = jnp.arange(1, N_SOFTMAX_HEADS + 1, dtype=jnp.float32)
    s = 2.0 ** (-8.0 * i / N_SOFTMAX_HEADS)
    return s[SWA_HEADS:], s[:SWA_HEADS]


def split_cols(t, sizes):
    out, start = [], 0
    for s in sizes:
        out.append(t[..., start:start + s])
        start += s
    return out


def swiglu(h, w_gate, w_up, w_down):
    return jnp.dot(jax.nn.silu(jnp.dot(h, w_gate)) * jnp.dot(h, w_up), w_down)


def banded_attention(q, k, v, slopes, window, sinks=None):
    B, S, H, d = q.shape
    G = k.shape[2]
    R = H // G
    nq = S // Q_BLOCK
    n_prev = -(-(window - 1) // Q_BLOCK)

    def band(t):
        tb = t.reshape(B, nq, Q_BLOCK, G, d)
        tb = jnp.pad(tb, ((0, 0), (n_prev, 0), (0, 0), (0, 0), (0, 0)))
        return jnp.concatenate([tb[:, i:i + nq] for i in range(n_prev + 1)], axis=2)

    kb, vb = band(k), band(v)
    qb = q.reshape(B, nq, Q_BLOCK, G, R, d)
    s = jnp.einsum('bnqgrd,bnkgd->bgrnqk', qb, kb).astype(jnp.float32) * (d ** -0.5)
    qpos = jnp.arange(nq)[:, None] * Q_BLOCK + jnp.arange(Q_BLOCK)[None]
    kpos = jnp.arange(nq)[:, None] * Q_BLOCK - n_prev * Q_BLOCK + jnp.arange((n_prev + 1) * Q_BLOCK)[None]
    dist = qpos[:, :, None] - kpos[:, None, :]
    mask = (dist >= 0) & (dist < window) & (kpos[:, None, :] >= 0)
    logits = s - slopes.astype(jnp.float32).reshape(G, R, 1, 1, 1) * dist.astype(jnp.float32)
    logits = jnp.where(mask, logits, NEG_INF)
    if sinks is not None:
        sink = jnp.broadcast_to(sinks.astype(jnp.float32).reshape(G, R, 1, 1, 1), logits.shape[:-1] + (1,))
        p = jax.nn.softmax(jnp.concatenate([logits, sink], axis=-1), axis=-1)[..., :-1]
    else:
        p = jax.nn.softmax(logits, axis=-1)
    o = jnp.einsum('bgrnqk,bnkgd->bnqgrd', p.astype(v.dtype), vb)
    return o.reshape(B, S, H, d)


def nsa_mixer(q, k_cmp, v_cmp, k_slc, v_slc, k_win, v_win, gate_logits,
              pos_k, pos_v, w1_k, w2_k, w1_v, w2_v, slopes):
    B, S, H, d = q.shape
    f32 = jnp.float32
    scale = d ** -0.5
    t_pos = jnp.arange(S)

    n_cmp = (S - CMP_BLOCK) // CMP_STRIDE + 1
    win_idx = jnp.arange(n_cmp)[:, None] * CMP_STRIDE + jnp.arange(CMP_BLOCK)[None]

    def compress(t, pos, w1, w2):
        blocks = (t[:, win_idx] + pos).reshape(B, n_cmp, CMP_BLOCK * d)
        return jnp.dot(jax.nn.silu(jnp.dot(blocks, w1)), w2)

    kc = compress(k_cmp, pos_k, w1_k, w2_k)
    vc = compress(v_cmp, pos_v, w1_v, w2_v)
    c_end = jnp.arange(n_cmp) * CMP_STRIDE + CMP_BLOCK - 1
    c_dist = (t_pos[:, None] - c_end[None]).astype(f32)
    sc = jnp.einsum('bthd,bnd->bhtn', q, kc).astype(f32) * scale - slopes[:, None, None] * c_dist
    p_cmp = masked_softmax(sc, c_dist >= 0) * (t_pos >= CMP_BLOCK - 1).astype(f32)[:, None]
    o_cmp = jnp.einsum('bhtn,bnd->bthd', p_cmp.astype(vc.dtype), vc)

    n_sel = S // SEL_BLOCK
    top_n = min(SEL_TOPN, n_sel)
    c_start = jnp.arange(n_cmp) * CMP_STRIDE
    s_start = jnp.arange(n_sel) * SEL_BLOCK
    overlap = jnp.clip(jnp.minimum(c_start[:, None] + CMP_BLOCK, s_start[None] + SEL_BLOCK)
                       - jnp.maximum(c_start[:, None], s_start[None]), 0).astype(f32) / CMP_BLOCK
    importance = jnp.einsum('bhtn,nj->btj', p_cmp, overlap)
    cur = t_pos // SEL_BLOCK
    blk = jnp.arange(n_sel)
    valid = blk[None] <= cur[:, None]
    forced = (blk[None] == 0) | (blk[None] == cur[:, None]) | (blk[None] == cur[:, None] - 1)
    score = jnp.where(valid, importance + jnp.where(forced, FORCE_BONUS, 0.0), -1.0)
    _, sel = lax.top_k(score, top_n)
    tok = (sel[..., None] * SEL_BLOCK + jnp.arange(SEL_BLOCK)).reshape(B, S, top_n * SEL_BLOCK)

    nq = S // Q_BLOCK
    qb = q.reshape(B, nq, Q_BLOCK, H, d).transpose(1, 0, 2, 3, 4)
    tokb = tok.reshape(B, nq, Q_BLOCK, top_n * SEL_BLOCK).transpose(1, 0, 2, 3)
    posb = t_pos.reshape(nq, Q_BLOCK)
    bidx = jnp.arange(B)[:, None, None]

    def sel_block(args):
        qi, ti, pi = args
        kg = k_slc[bidx, ti]
        vg = v_slc[bidx, ti]
        dist = (pi[None, :, None] - ti).astype(f32)
        s = jnp.einsum('bqhd,bqkd->bhqk', qi, kg).astype(f32) * scale - slopes[None, :, None, None] * dist[:, None]
        p = masked_softmax(s, (dist >= 0)[:, None])
        return jnp.einsum('bhqk,bqkd->bqhd', p.astype(vg.dtype), vg)

    o_slc = lax.map(sel_block, (qb, tokb, posb)).transpose(1, 0, 2, 3, 4).reshape(B, S, H, d)

    o_win = banded_attention(q, k_win[:, :, None], v_win[:, :, None], slopes, NSA_WINDOW)

    g = jax.nn.sigmoid(gate_logits.astype(f32)).reshape(B, S, 3, H)
    o = g[:, :, 0, :, None] * o_cmp + g[:, :, 1, :, None] * o_slc + g[:, :, 2, :, None] * o_win
    return o.astype(q.dtype).reshape(B, S, H * d)


def chunk_gated_delta(q, k, v, beta, g):
    B, H, S, d = q.shape
    C = GDN_CHUNK
    N = S // C
    q, k, v = (t.reshape(B, H, N, C, d) for t in (q, k, v))
    beta = beta.reshape(B, H, N, C)
    gc = jnp.cumsum(g.reshape(B, H, N, C), axis=-1)
    i = jnp.arange(C)
    lower_incl = i[:, None] >= i[None]
    strict = i[:, None] > i[None]
    decay = jnp.exp(jnp.where(lower_incl, gc[..., :, None] - gc[..., None, :], NEG_INF))
    kb = k * beta[..., None]
    L = jnp.where(strict, jnp.einsum('bhncd,bhnjd->bhncj', kb, k) * decay, 0.0)
    a_mat = L + jnp.eye(C, dtype=jnp.float32)
    rhs = jnp.concatenate([v * beta[..., None], kb * jnp.exp(gc)[..., None]], axis=-1)
    sol = lax.linalg.triangular_solve(a_mat, rhs, left_side=True, lower=True, unit_diagonal=True)
    u, w = sol[..., :d], sol[..., d:]
    attn = jnp.where(lower_incl, jnp.einsum('bhncd,bhnjd->bhncj', q, k) * decay, 0.0)
    q_dec = q * jnp.exp(gc)[..., None]
    g_last = gc[..., -1]
    k_dec = k * jnp.exp(g_last[..., None] - gc)[..., None]

    def step(state, inp):
        u_c, w_c, q_c, k_c, a_c, gl = inp
        v_new = u_c - jnp.einsum('bhck,bhkv->bhcv', w_c, state)
        o_c = jnp.einsum('bhck,bhkv->bhcv', q_c, state) + jnp.einsum('bhcj,bhjv->bhcv', a_c, v_new)
        state = state * jnp.exp(gl)[..., None, None] + jnp.einsum('bhck,bhcv->bhkv', k_c, v_new)
        return state, o_c

    xs = tuple(jnp.moveaxis(t, 2, 0) for t in (u, w, q_dec, k_dec, attn, g_last))
    state0 = jnp.zeros((B, H, d, d), jnp.float32)
    _, o = lax.scan(step, state0, xs)
    return jnp.moveaxis(o, 0, 2).reshape(B, H, S, d)


def gdn_mixer(q, k, v, z, a, b, conv_w, a_log, dt_bias, norm_w):
    B, S, _ = q.shape
    H, d = GDN_HEADS, HEAD_DIM
    f32 = jnp.float32
    qkv = jnp.concatenate([q, k, v], axis=-1)
    ch = qkv.shape[-1]
    qkv = lax.conv_general_dilated(qkv, conv_w[:, None, :].astype(qkv.dtype), window_strides=(1,),
                                   padding=[(GDN_CONV - 1, 0)], dimension_numbers=('NWC', 'WIO', 'NWC'),
                                   feature_group_count=ch)
    qkv = jax.nn.silu(qkv).astype(f32)
    qc, kc, vc = jnp.split(qkv, 3, axis=-1)

    def heads(t):
        return t.reshape(B, S, H, d).transpose(0, 2, 1, 3)

    qh = l2norm(heads(qc)) * (d ** -0.5)
    kh = l2norm(heads(kc))
    vh = heads(vc)
    beta = jax.nn.sigmoid(b.astype(f32)).transpose(0, 2, 1)
    g = -(jnp.exp(a_log.astype(f32)) * jax.nn.softplus(a.astype(f32) + dt_bias.astype(f32))).transpose(0, 2, 1)
    o = chunk_gated_delta(qh, kh, vh, beta, g).transpose(0, 2, 1, 3)
    o = o * lax.rsqrt(jnp.mean(o * o, axis=-1, keepdims=True) + NORM_EPS) * norm_w.astype(f32)
    o = o * jax.nn.silu(z.reshape(B, S, H, d).astype(f32))
    return o.reshape(B, S, H * d).astype(z.dtype)


def hybrid_mixer(h, w_in, pos_k, pos_v, w1_k, w2_k, w1_v, w2_v, conv_w, a_log, dt_bias,
                 gdn_norm_w, sinks, w_out, nsa_slopes, swa_slopes):
    B, S, _ = h.shape
    (nsa_q, kc, vc, ksl, vsl, kw, vw, nsa_g, gq, gk, gv, gz, ga, gb, sq, sk, sv) = split_cols(jnp.dot(h, w_in), IN_SPLIT_SIZES)
    o_nsa = nsa_mixer(nsa_q.reshape(B, S, NSA_HEADS, HEAD_DIM), kc, vc, ksl, vsl, kw, vw, nsa_g,
                      pos_k, pos_v, w1_k, w2_k, w1_v, w2_v, nsa_slopes)
    o_gdn = gdn_mixer(gq, gk, gv, gz, ga, gb, conv_w, a_log, dt_bias, gdn_norm_w)
    o_swa = banded_attention(sq.reshape(B, S, SWA_HEADS, HEAD_DIM), sk.reshape(B, S, SWA_KV_HEADS, HEAD_DIM),
                             sv.reshape(B, S, SWA_KV_HEADS, HEAD_DIM), swa_slopes, SWA_WINDOW, sinks).reshape(B, S, SWA_WIDTH)
    return jnp.dot(jnp.concatenate([o_nsa, o_gdn, o_swa], axis=-1), w_out)


def moe_swiglu(h, router, w_gate, w_up, w_down):
    N, D = h.shape
    logits = jnp.dot(h, router).astype(jnp.float32)
    top_logit, top_idx = lax.top_k(logits, TOP_K)
    gate = jax.nn.softmax(top_logit, axis=-1)
    A = N * TOP_K
    e_flat = top_idx.reshape(A)
    tok_flat = jnp.arange(A, dtype=jnp.int32) // TOP_K
    gate_flat = gate.reshape(A)
    order = jnp.argsort(e_flat)
    se, stok, sgate = e_flat[order], tok_flat[order], gate_flat[order]
    counts = jnp.zeros((N_EXPERTS,), jnp.int32).at[e_flat].add(1)
    starts = jnp.cumsum(counts) - counts
    padded = (counts + MOE_BLOCK - 1) // MOE_BLOCK * MOE_BLOCK
    pad_end = jnp.cumsum(padded)
    pad_start = pad_end - padded
    dest = pad_start[se] + jnp.arange(A, dtype=jnp.int32) - starts[se]
    n_blocks = -(-A // MOE_BLOCK) + N_EXPERTS
    P = n_blocks * MOE_BLOCK
    row_tok = jnp.zeros((P,), jnp.int32).at[dest].set(stok)
    row_gate = jnp.zeros((P,), jnp.float32).at[dest].set(sgate)
    blk_expert = jnp.clip(jnp.searchsorted(pad_end, jnp.arange(n_blocks, dtype=jnp.int32) * MOE_BLOCK, side='right'),
                          0, N_EXPERTS - 1)
    xs = h[row_tok].reshape(n_blocks, MOE_BLOCK, D)

    def expert_block(args):
        xb, e = args
        return swiglu(xb, w_gate[e], w_up[e], w_down[e])

    ys = lax.map(expert_block, (xs, blk_expert)).reshape(P, D)
    ys = ys * row_gate[:, None].astype(ys.dtype)
    return jax.ops.segment_sum(ys, row_tok, num_segments=N)


def setup_inputs(seed: int = 0) -> dict:
    key = jax.random.key(seed)
    ks = jax.random.split(key, 24)
    f32 = jnp.float32

    def nrm(k, shape, fan_in):
        return jax.random.normal(k, shape, f32) * (fan_in ** -0.5)

    def gain(k, shape):
        return 1.0 + 0.01 * jax.random.normal(k, shape, f32)

    dt = jnp.exp(jax.random.uniform(ks[11], (DEPTH, GDN_HEADS), f32, math.log(1e-3), math.log(1e-1)))
    return {
        "x": jax.random.normal(ks[0], (BATCH, SEQ, D_MODEL), f32),
        "attn_norm": gain(ks[1], (DEPTH, D_MODEL)),
        "w_in": nrm(ks[2], (DEPTH, D_MODEL, D_IN), D_MODEL),
        "cmp_pos_k": 0.1 * jax.random.normal(ks[3], (DEPTH, CMP_BLOCK, HEAD_DIM), f32),
        "cmp_pos_v": 0.1 * jax.random.normal(ks[4], (DEPTH, CMP_BLOCK, HEAD_DIM), f32),
        "cmp_w1_k": nrm(ks[5], (DEPTH, CMP_BLOCK * HEAD_DIM, CMP_HIDDEN), CMP_BLOCK * HEAD_DIM),
        "cmp_w2_k": nrm(ks[6], (DEPTH, CMP_HIDDEN, HEAD_DIM), CMP_HIDDEN),
        "cmp_w1_v": nrm(ks[7], (DEPTH, CMP_BLOCK * HEAD_DIM, CMP_HIDDEN), CMP_BLOCK * HEAD_DIM),
        "cmp_w2_v": nrm(ks[8], (DEPTH, CMP_HIDDEN, HEAD_DIM), CMP_HIDDEN),
        "gdn_conv_w": nrm(ks[9], (DEPTH, GDN_CONV, 3 * GDN_WIDTH), GDN_CONV),
        "gdn_a_log": jnp.log(jax.random.uniform(ks[10], (DEPTH, GDN_HEADS), f32, 1.0, 16.0)),
        "gdn_dt_bias": dt + jnp.log(-jnp.expm1(-dt)),
        "gdn_norm_w": gain(ks[12], (DEPTH, HEAD_DIM)),
        "swa_sinks": 0.5 * jax.random.normal(ks[13], (DEPTH, SWA_HEADS), f32),
        "w_out": nrm(ks[14], (DEPTH, MIX_WIDTH, D_MODEL), MIX_WIDTH),
        "ffn_norm": gain(ks[15], (DEPTH, D_MODEL)),
        "dense_w_gate": nrm(ks[16], (N_DENSE, D_MODEL, D_FF), D_MODEL),
        "dense_w_up": nrm(ks[17], (N_DENSE, D_MODEL, D_FF), D_MODEL),
        "dense_w_down": nrm(ks[18], (N_DENSE, D_FF, D_MODEL), D_FF),
        "moe_router": nrm(ks[19], (N_MOE, D_MODEL, N_EXPERTS), D_MODEL),
        "moe_w_gate": nrm(ks[20], (N_MOE, N_EXPERTS, D_MODEL, EXPERT_FF), D_MODEL),
        "moe_w_up": nrm(ks[21], (N_MOE, N_EXPERTS, D_MODEL, EXPERT_FF), D_MODEL),
        "moe_w_down": nrm(ks[22], (N_MOE, N_EXPERTS, EXPERT_FF, D_MODEL), EXPERT_FF),
        "final_norm": gain(ks[23], (D_MODEL,)),
    }


def reference(x, attn_norm, w_in, cmp_pos_k, cmp_pos_v, cmp_w1_k, cmp_w2_k, cmp_w1_v, cmp_w2_v,
              gdn_conv_w, gdn_a_log, gdn_dt_bias, gdn_norm_w, swa_sinks, w_out, ffn_norm,
              dense_w_gate, dense_w_up, dense_w_down, moe_router, moe_w_gate, moe_w_up, moe_w_down,
              final_norm):
    nsa_slopes, swa_slopes = alibi_slopes()
    B, S, D = x.shape
    for layer in range(DEPTH):
        h = rmsnorm(x, attn_norm[layer])
        x = x + hybrid_mixer(h, w_in[layer], cmp_pos_k[layer], cmp_pos_v[layer], cmp_w1_k[layer], cmp_w2_k[layer],
                             cmp_w1_v[layer], cmp_w2_v[layer], gdn_conv_w[layer], gdn_a_log[layer],
                             gdn_dt_bias[layer], gdn_norm_w[layer], swa_sinks[layer], w_out[layer],
                             nsa_slopes, swa_slopes)
        h = rmsnorm(x, ffn_norm[layer]).reshape(B * S, D)
        i = layer // 2
        if layer % 2 == 0:
            f = swiglu(h, dense_w_gate[i], dense_w_up[i], dense_w_down[i])
        else:
            f = moe_swiglu(h, moe_router[i], moe_w_gate[i], moe_w_up[i], moe_w_down[i])
        x = x + f.reshape(B, S, D)
    return rmsnorm(x, final_norm)
```

```python
import contextlib
import numpy as np
import concourse.bass as bass
import concourse.mybir as mybir
from concourse.bass_utils import run_bass_kernel_spmd

F32 = mybir.dt.float32
BF16 = mybir.dt.bfloat16
AF = mybir.ActivationFunctionType
ALU = mybir.AluOpType
AX = mybir.AxisListType

ENGS = ("pe", "act", "dve", "pool", "sp")

D = 2048
S = 2048
NB = 4
HD = 128
DFF = 7168
NEXP = 8
EPS = 1e-6
KC = D // 128


class Prog:
    def __init__(self, nc, n_dma_sems=40, same_engine_sync=True, epoch=30000):
        self.nc = nc
        self.stack = contextlib.ExitStack()
        self.ops = {e: [] for e in ENGS}
        self.same = same_engine_sync
        self.epoch = epoch
        self.cnt = {e: 0 for e in ENGS}
        self.esem = {}
        for e in ("pe", "act", "dve", "pool"):
            self.esem[e] = nc.alloc_semaphore(f"s_{e}_0")
        self.nsem = {e: 1 for e in ENGS}
        self.dma_sems = [nc.alloc_semaphore(f"s_dma_{i}") for i in range(n_dma_sems)]
        self.dma_val = [0] * n_dma_sems
        self.dma_rr = 0
        self.known = {e: {} for e in ENGS}
        self.last_w = {}
        self.readers = {}
        self.n_ops = 0
        self.limit = None
        self.serial = False
        self.prev = None

    def sbuf(self, name, shape, dtype):
        return self.stack.enter_context(self.nc.sbuf_tensor("sb_" + name, list(shape), dtype))

    def psum(self, name, shape, dtype=F32):
        return self.stack.enter_context(self.nc.psum_tensor("pt_" + name, list(shape), dtype))

    def _need(self, eng, comp, waits):
        if comp is None:
            return
        sem, val, ceng = comp
        if ceng == eng and (eng == "pe" or not self.same):
            return
        k = self.known[eng]
        if k.get(sem.name, 0) >= val:
            return
        k[sem.name] = val
        waits.append((sem, val))

    def op(self, eng, fn, reads=(), writes=(), dma=False):
        if self.limit is not None and self.n_ops >= self.limit:
            return None
        excl = [b for b in reads if isinstance(b, str) and b.startswith("ps")]
        if excl:
            reads = [b for b in reads if b not in excl]
            writes = list(writes) + excl
        waits = []
        if self.serial and self.prev is not None:
            self._need(eng, self.prev, waits)
        for b in reads:
            self._need(eng, self.last_w.get(b), waits)
        for b in writes:
            self._need(eng, self.last_w.get(b), waits)
            for r in self.readers.get(b, ()):
                self._need(eng, r, waits)
        if dma:
            i = self.dma_rr
            self.dma_rr = (i + 1) % len(self.dma_sems)
            sem = self.dma_sems[i]
            if self.dma_val[i] > 0:
                self._need(eng, (sem, self.dma_val[i], "dma"), waits)
            self.dma_val[i] += 16
            comp = (sem, self.dma_val[i], "dma")
            inc = (sem, 16)
        else:
            if self.cnt[eng] >= self.epoch:
                self.esem[eng] = self.nc.alloc_semaphore(f"s_{eng}_{self.nsem[eng]}")
                self.nsem[eng] += 1
                self.cnt[eng] = 0
            self.cnt[eng] += 1
            comp = (self.esem[eng], self.cnt[eng], eng)
            inc = (self.esem[eng], 1)
        for b in reads:
            self.readers.setdefault(b, []).append(comp)
        for b in writes:
            self.last_w[b] = comp
            self.readers[b] = []
        self.ops[eng].append((waits, fn, inc))
        self.n_ops += 1
        self.prev = comp
        return comp

    def dma(self, out, in_, reads=(), writes=(), eng="sp", **kw):
        return self.op(eng, lambda e: e.dma_start(out=out, in_=in_, **kw), reads, writes, dma=True)

    def mm(self, out, lhsT, rhs, start, stop, reads=(), writes=()):
        return self.op("pe", lambda e: e.matmul(out, lhsT, rhs, start=start, stop=stop), reads, writes)

    def tr(self, out, in_, ident, reads=(), writes=()):
        return self.op("pe", lambda e: e.transpose(out, in_, ident), reads, writes)

    def act(self, out, in_, func, reads=(), writes=(), **kw):
        return self.op("act", lambda e: e.activation(out, in_, func, **kw), reads, writes)

    def tt(self, eng, out, in0, in1, op, reads=(), writes=()):
        return self.op(eng, lambda e: e.tensor_tensor(out, in0, in1, op), reads, writes)

    def ts(self, eng, out, in0, s1, s2, op0, op1=None, reads=(), writes=()):
        if op1 is None:
            return self.op(eng, lambda e: e.tensor_scalar(out, in0, s1, s2, op0), reads, writes)
        return self.op(eng, lambda e: e.tensor_scalar(out, in0, s1, s2, op0, op1), reads, writes)

    def stt(self, eng, out, in0, scalar, in1, op0, op1, reads=(), writes=()):
        return self.op(eng, lambda e: e.scalar_tensor_tensor(out, in0, scalar, in1, op0, op1), reads, writes)

    def cp(self, eng, out, in_, reads=(), writes=()):
        if eng == "act":
            return self.op("act", lambda e: e.copy(out, in_), reads, writes)
        return self.op(eng, lambda e: e.tensor_copy(out, in_), reads, writes)

    def final_wait(self, eng, comps):
        waits = []
        for c in comps:
            if c is not None:
                self._need(eng, c, waits)
        self.ops[eng].append((waits, None, None))

    def emit(self):
        nc = self.nc
        ops = self.ops

        def run(name, eng):
            for waits, fn, inc in ops[name]:
                for sem, val in waits:
                    eng.wait_ge(sem, val)
                if fn is None:
                    continue
                ins = fn(eng)
                if inc is not None:
                    ins.then_inc(inc[0], inc[1])

        with nc.Block() as block:
            @block.tensor
            def _(e):
                run("pe", e)

            @block.scalar
            def _(e):
                run("act", e)

            @block.vector
            def _(e):
                run("dve", e)

            @block.gpsimd
            def _(e):
                run("pool", e)

            @block.sync
            def _(e):
                run("sp", e)
        self.stack.close()


def emit_norm_transpose(p, x_ap, xkey, wB, hn, ss, ident, pst, pstk, hT, tcol, hT32=None):
    p.act(hn[:], x_ap, AF.Square, [xkey], ["hn", "ss0"], accum_out=ss[:, 0:1])
    p.act(ss[:, 1:2], ss[:, 0:1], AF.Ln, ["ss0"], ["ss1"], bias=EPS, scale=1.0 / D)
    p.act(ss[:, 1:2], ss[:, 1:2], AF.Exp, ["ss1"], ["ss1"], scale=-0.5)
    p.stt("dve", hn[:], x_ap, ss[:, 1:2], wB[:], ALU.mult, ALU.mult, [xkey, "ss1", "wB"], ["hn"])
    for g in range(4):
        ps = pst[g % 2]
        pk = pstk[g % 2]
        for j in range(4):
            kc = g * 4 + j
            p.tr(ps[:, j * 128:(j + 1) * 128], hn[:, kc * 128:(kc + 1) * 128], ident[:], ["hn", "ident"], [pk])
        dst = hT[:, g * 4:(g + 1) * 4, tcol:tcol + 128]
        src = ps[:].rearrange("p (j t) -> p j t", j=4)
        p.cp("dve" if g % 2 == 0 else "act", dst, src, [pk], [("hT", tcol)])
        if hT32 is not None:
            p.cp("act" if g % 2 == 0 else "dve", hT32[:, g * 4:(g + 1) * 4, :], src, [pk], ["hT32"])


NT_F = 8
FC = 256


def build_ffn(n_exp, final):
    moe = n_exp > 1
    nc = bass.Bass("TRN2", target_bir_lowering=False)
    dt = lambda n, sh, kind="ExternalInput": nc.dram_tensor(n, sh, F32, kind=kind).ap()
    xin = dt("xin", [NT_F, 128, D])
    oT_d = dt("oT", [128, KC, NT_F * 128])
    wout_d = dt("wout", [128, KC, D])
    nw_d = dt("normw", [128, D])
    wg_d = dt("wg", [n_exp, 128, KC, DFF])
    wu_d = dt("wu", [n_exp, 128, KC, DFF])
    wd_d = dt("wd", [n_exp, 128, DFF // 128, D])
    id_d = dt("ident", [128, 128])
    if moe:
        rt_d = dt("router", [128, KC, NEXP])
    if final:
        fw_d = dt("fnormw", [128, D])
    xout = dt("xout", [NT_F, 128, D], kind="ExternalOutput")

    p = Prog(nc)
    NTOK = NT_F * 128
    xres = p.sbuf("xres", [128, NT_F, D], F32)
    hT = p.sbuf("hT", [128, KC, NTOK], BF16)
    wa = [[p.sbuf(f"wa{b}{k}", [128, KC, FC], BF16) for k in range(2)] for b in range(2)]
    wdn = [p.sbuf(f"wdn{b}", [128, FC // 128, D], BF16) for b in range(2)]
    wB = p.sbuf("wB", [128, D], F32)
    hn = p.sbuf("hn", [128, D], F32)
    ss = p.sbuf("ss", [128, 2], F32)
    ident = p.sbuf("ident", [128, 128], F32)
    aT = [p.sbuf(f"aT{b}", [128, FC // 128, NTOK], BF16) for b in range(2)]
    sg = [p.sbuf(f"sg{b}", [128, 512], F32) for b in range(2)]
    ps = [p.psum(f"ps{i}", [128, 512], F32) for i in range(8)]
    if moe:
        hT32 = p.sbuf("hT32", [128, KC, 128], F32)
        rt = p.sbuf("rt", [128, KC, NEXP], F32)
        gate = p.sbuf("gate", [128, NT_F, NEXP], F32)
        lg = p.sbuf("lg", [128, NEXP], F32)
        top = p.sbuf("top", [128, 8], F32)
        gsc = p.sbuf("gsc", [128, 4], F32)
        m1 = p.sbuf("m1", [128, NEXP], F32)
        p.dma(rt[:], rt_d, writes=["rt"])

    p.dma(ident[:], id_d, writes=["ident"])
    p.dma(wB[:], nw_d, writes=["wB"])
    for t in range(NT_F):
        p.dma(xres[:, t, :], xin[t], writes=[("x", t)])
    for g in range(4):
        p.dma(hT[:, g * 4:(g + 1) * 4, :], oT_d[:, g * 4:(g + 1) * 4, :], writes=[("hT", c * 128) for c in range(NT_F)], eng="pool")

    nq = 0
    for cg in range(D // FC):
        wb = wa[cg % 2][0]
        wk = f"wa{cg % 2}0"
        p.dma(wb[:], wout_d[:, :, cg * FC:(cg + 1) * FC], writes=[wk], eng="pool")
        for t in range(NT_F):
            pp = ps[nq % 4]
            pk = f"ps{nq % 4}"
            nq += 1
            for kc in range(KC):
                p.mm(pp[:, 0:FC], hT[:, kc, t * 128:(t + 1) * 128], wb[:, kc, :], kc == 0, kc == KC - 1,
                     [("hT", t * 128), wk], [pk])
            xs = xres[:, t, cg * FC:(cg + 1) * FC]
            p.tt("dve", xs, pp[:, 0:FC], xs, ALU.add, [pk, ("x", t)], [("x", t)])

    for t in range(NT_F):
        emit_norm_transpose(p, xres[:, t, :], ("x", t), wB, hn, ss, ident, [ps[4], ps[5]], ["ps4", "ps5"], hT, t * 128,
                            hT32=hT32 if moe else None)
        if moe:
            for kc in range(KC):
                p.mm(ps[6][:, 0:NEXP], hT32[:, kc, :], rt[:, kc, :], kc == 0, kc == KC - 1, ["hT32", "rt"], ["ps6"])
            p.cp("dve", lg[:], ps[6][:, 0:NEXP], ["ps6"], ["lg"])
            p.op("dve", lambda e: e.max(top[:], lg[:]), ["lg"], ["top"])
            p.tt("dve", gsc[:, 0:1], top[:, 1:2], top[:, 0:1], ALU.subtract, ["top"], ["gsc"])
            p.act(gsc[:, 1:2], gsc[:, 0:1], AF.Exp, ["gsc"], ["gsc1"])
            p.ts("dve", gsc[:, 1:2], gsc[:, 1:2], 1.0, None, ALU.add, None, ["gsc1"], ["gsc1"])
            p.op("dve", lambda e: e.reciprocal(gsc[:, 2:3], gsc[:, 1:2]), ["gsc1"], ["gsc2"])
            p.ts("dve", gsc[:, 3:4], gsc[:, 2:3], -1.0, 1.0, ALU.mult, ALU.add, ["gsc2"], ["gsc3"])
            p.ts("dve", m1[:], lg[:], top[:, 0:1], gsc[:, 2:3], ALU.is_equal, ALU.mult, ["lg", "top", "gsc2"], ["m1"])
            p.ts("dve", gate[:, t, :], lg[:], top[:, 1:2], gsc[:, 3:4], ALU.is_equal, ALU.mult, ["lg", "top", "gsc3"], [("gate", t)])
            p.tt("dve", gate[:, t, :], gate[:, t, :], m1[:], ALU.add, [("gate", t), "m1"], [("gate", t)])

    nchunk = DFF // FC
    it = 0
    for ex in range(n_exp):
        for fc in range(nchunk):
            b = it % 2
            it += 1
            wgb, wub, wdb = wa[b][0], wa[b][1], wdn[b]
            kg, ku, kd = f"wa{b}0", f"wa{b}1", f"wdn{b}"
            f0 = fc * FC
            p.dma(wgb[:], wg_d[ex, :, :, f0:f0 + FC], writes=[kg], eng="pool")
            p.dma(wub[:], wu_d[ex, :, :, f0:f0 + FC], writes=[ku], eng="pool")
            p.dma(wdb[:], wd_d[ex, :, fc * (FC // 128):(fc + 1) * (FC // 128), :], writes=[kd], eng="pool")
            ab = aT[b]
            ak = f"aT{b}"
            for fs in range(FC // 128):
                for tg in range(NTOK // 512):
                    pg, pu = ps[(nq % 2) * 2], ps[(nq % 2) * 2 + 1]
                    kpg, kpu = f"ps{(nq % 2) * 2}", f"ps{(nq % 2) * 2 + 1}"
                    sgb = sg[nq % 2]
                    ksg = f"sg{nq % 2}"
                    nq += 1
                    hk = [("hT", c * 128) for c in range(tg * 4, tg * 4 + 4)]
                    for kc in range(KC):
                        p.mm(pg[:], wgb[:, kc, fs * 128:(fs + 1) * 128], hT[:, kc, tg * 512:(tg + 1) * 512],
                             kc == 0, kc == KC - 1, [kg] + hk, [kpg])
                    for kc in range(KC):
                        p.mm(pu[:], wub[:, kc, fs * 128:(fs + 1) * 128], hT[:, kc, tg * 512:(tg + 1) * 512],
                             kc == 0, kc == KC - 1, [ku] + hk, [kpu])
                    p.act(sgb[:], pg[:], AF.Silu, [kpg], [ksg])
                    p.tt("dve", ab[:, fs, tg * 512:(tg + 1) * 512], pu[:], sgb[:], ALU.mult, [kpu, ksg], [(ak, fs, tg)])
            for t in range(NT_F):
                for cg in range(D // 512):
                    pp = ps[4 + nq % 4]
                    pk = f"ps{4 + nq % 4}"
                    nq += 1
                    nfs = FC // 128
                    for fs in range(nfs):
                        p.mm(pp[:], ab[:, fs, t * 128:(t + 1) * 128], wdb[:, fs, cg * 512:(cg + 1) * 512],
                             fs == 0, fs == nfs - 1, [(ak, fs, t // 4), kd], [pk])
                    xs = xres[:, t, cg * 512:(cg + 1) * 512]
                    if moe:
                        p.stt("dve", xs, pp[:], gate[:, t, ex:ex + 1], xs, ALU.mult, ALU.add,
                              [pk, ("x", t), ("gate", t)], [("x", t)])
                    else:
                        p.tt("dve", xs, pp[:], xs, ALU.add, [pk, ("x", t)], [("x", t)])

    fin = []
    if final:
        p.dma(wB[:], fw_d, writes=["wB"])
    for t in range(NT_F):
        if final:
            p.act(hn[:], xres[:, t, :], AF.Square, [("x", t)], ["hn", "ss0"], accum_out=ss[:, 0:1])
            p.act(ss[:, 1:2], ss[:, 0:1], AF.Ln, ["ss0"], ["ss1"], bias=EPS, scale=1.0 / D)
            p.act(ss[:, 1:2], ss[:, 1:2], AF.Exp, ["ss1"], ["ss1"], scale=-0.5)
            p.stt("dve", xres[:, t, :], xres[:, t, :], ss[:, 1:2], wB[:], ALU.mult, ALU.mult,
                  [("x", t), "ss1", "wB"], [("x", t)])
        fin.append(p.dma(xout[t], xres[:, t, :], reads=[("x", t)]))
    p.final_wait("sp", fin)
    p.emit()
    return nc


NT = S // 128
SCALE = HD ** -0.5
VW = 144


def mixer_prologue(nc, p, x_d, nw_d, id_d):
    hT = p.sbuf("hT", [128, KC, S], BF16)
    xt = p.sbuf("xt", [128, D], F32)
    wB = p.sbuf("wB", [128, D], F32)
    hn = p.sbuf("hn", [128, D], F32)
    ss = p.sbuf("ss", [128, 2], F32)
    ident = p.sbuf("ident", [128, 128], F32)
    ps = [p.psum(f"ps{i}", [128, 512], F32) for i in range(8)]
    p.dma(ident[:], id_d, writes=["ident"])
    p.dma(wB[:], nw_d, writes=["wB"])
    for t in range(NT):
        p.dma(xt[:], x_d[t], writes=["xt"])
        emit_norm_transpose(p, xt[:], "xt", wB, hn, ss, ident, [ps[0], ps[1]], ["ps0", "ps1"], hT, t * 128)
    return hT, ident, ps, hn


def hkeys(t0, t1):
    return [("hT", t * 128) for t in range(t0, t1)]


def proj_fm(p, ps, w, wkey, c0, ncol, hT, dst_fn, dkey_fn, cnt):
    for tg in range(S // 512):
        pp = ps[cnt[0] % 2]
        pk = f"ps{cnt[0] % 2}"
        cnt[0] += 1
        for kc in range(KC):
            p.mm(pp[0:ncol, :], w[:, kc, c0:c0 + ncol], hT[:, kc, tg * 512:(tg + 1) * 512], kc == 0, kc == KC - 1,
                 [wkey] + hkeys(tg * 4, tg * 4 + 4), [pk])
        p.cp("act" if cnt[0] % 2 else "dve", dst_fn(tg), pp[0:ncol, :], [pk], [dkey_fn(tg)])


def proj_tm(p, ps, w, wkey, c0, ncol, hT, t, dst, dkey, cnt, ntok=128, tok0=None):
    tok0 = t * 128 if tok0 is None else tok0
    pp = ps[cnt[0] % 2]
    pk = f"ps{cnt[0] % 2}"
    cnt[0] += 1
    for kc in range(KC):
        p.mm(pp[0:ntok, 0:ncol], hT[:, kc, tok0:tok0 + ntok], w[:, kc, c0:c0 + ncol], kc == 0, kc == KC - 1,
             [wkey, ("hT", (tok0 // 128) * 128)], [pk])
    p.cp("act" if cnt[0] % 2 else "dve", dst, pp[0:ntok, 0:ncol], [pk], [dkey])


def emit_band(p, qt, nprev, qT, qkey, kT, kkey, vaug, vkey, bias, maskC, maskE, sT, sTk, accs, acck, E32, Em, cnt):
    kts = [kt for kt in range(qt - nprev, qt + 1) if kt >= 0]
    for kt in kts:
        rel = qt - kt
        p.mm(sT[:, 0:256], kT[:, kt * 128:(kt + 1) * 128],
             qT[:, 0:2, qt * 128:(qt + 1) * 128], True, True, [kkey, qkey], [sTk])
        for h in range(2):
            b = cnt[0] % 2
            cnt[0] += 1
            need_mask = (rel == 0) or (rel == nprev)
            if need_mask:
                p.act(E32[b][:], sT[:, h * 128:(h + 1) * 128], AF.Exp, [sTk, "bias"], [f"E32{b}"],
                      bias=bias[:, rel, h:h + 1], scale=SCALE)
                m = maskC if rel == 0 else maskE
                p.tt("dve", Em[b][:], E32[b][:], m[:], ALU.mult, [f"E32{b}", "masks"], [f"Em{b}"])
            else:
                p.act(Em[b][:], sT[:, h * 128:(h + 1) * 128], AF.Exp, [sTk, "bias"], [f"Em{b}"],
                      bias=bias[:, rel, h:h + 1], scale=SCALE)
            p.mm(accs[h], Em[b][:], vaug[:, kt, 0:129], kt == kts[0], kt == kts[-1], [f"Em{b}", vkey], [acck[h]])


def build_swa():
    nc = bass.Bass("TRN2", target_bir_lowering=False)
    dt = lambda n, sh, kind="ExternalInput": nc.dram_tensor(n, sh, F32, kind=kind).ap()
    x_d = dt("x", [NT, 128, D])
    nw_d = dt("normw", [128, D])
    id_d = dt("ident", [128, 128])
    w_d = dt("w", [128, KC, 512])
    bias_d = dt("bias", [128, 2, 2])
    mask_d = dt("masks", [128, 2, 128])
    sinkf_d = dt("sinkf", [128, 2])
    sink_d = dt("sinks", [128, 2])
    out_d = dt("o", [NT, 128, 256], kind="ExternalOutput")
    p = Prog(nc)
    hT, ident, ps, hn = mixer_prologue(nc, p, x_d, nw_d, id_d)
    w = p.sbuf("w", [128, KC, 512], BF16)
    qT = p.sbuf("qT", [128, 2, S], BF16)
    kT = p.sbuf("kT", [128, S], BF16)
    vaug = p.sbuf("vaug", [128, NT, VW], BF16)
    bias = p.sbuf("bias", [128, 2, 2], F32)
    masks = p.sbuf("masks", [128, 2, 128], F32)
    sinkf = p.sbuf("sinkf", [128, 2], F32)
    sinks = p.sbuf("sinks", [128, 2], F32)
    E32 = [p.sbuf(f"E32{b}", [128, 128], F32) for b in range(2)]
    Em = [p.sbuf(f"Em{b}", [128, 128], BF16) for b in range(2)]
    osb = [p.sbuf(f"osb{b}", [128, 256], F32) for b in range(2)]
    den = p.sbuf("den", [128, 4], F32)
    p.dma(w[:], w_d, writes=["w"], eng="pool")
    p.dma(bias[:], bias_d, writes=["bias"])
    p.dma(masks[:], mask_d, writes=["masks"])
    p.dma(sinkf[:], sinkf_d, writes=["sinkf"])
    p.dma(sinks[:], sink_d, writes=["sinks"])
    p.op("pool", lambda e: e.memset(vaug[:, :, 128:VW], 1.0), [], ["vaug"])
    p.act(sinks[:], sinks[:], AF.Exp, ["sinks"], ["sinks"])
    p.tt("dve", sinkf[:], sinkf[:], sinks[:], ALU.mult, ["sinkf", "sinks"], ["sinkf"])
    cnt = [0]
    for h in range(2):
        proj_fm(p, ps, w, "w", h * 128, 128, hT, lambda tg, h=h: qT[:, h, tg * 512:(tg + 1) * 512], lambda tg: "qT", cnt)
    proj_fm(p, ps, w, "w", 256, 128, hT, lambda tg: kT[:, tg * 512:(tg + 1) * 512], lambda tg: "kT", cnt)
    for t in range(NT):
        proj_tm(p, ps, w, "w", 384, 128, hT, t, vaug[:, t, 0:128], "vaug", cnt)
    fin = []
    c2 = [0]
    for qt in range(NT):
        b = qt % 2
        sT, sTk = ps[2 + b], f"ps{2 + b}"
        accs = [ps[4 + 2 * b][:, 0:129], ps[5 + 2 * b][:, 0:129]]
        acck = [f"ps{4 + 2 * b}", f"ps{5 + 2 * b}"]
        emit_band(p, qt, 1, qT, "qT", kT, "kT", vaug, "vaug", bias, masks[:, 0, :], masks[:, 1, :], sT, sTk,
                  accs, acck, E32, Em, c2)
        for h in range(2):
            p.tt("dve", den[:, h:h + 1], accs[h][:, 128:129], sinkf[:, h:h + 1], ALU.add, [acck[h], "sinkf"], [("den", h)])
            p.op("dve", lambda e, h=h: e.reciprocal(den[:, 2 + h:3 + h], den[:, h:h + 1]), [("den", h)], [("rden", h)])
            p.ts("dve", osb[b][:, h * 128:(h + 1) * 128], accs[h][:, 0:128], den[:, 2 + h:3 + h], None, ALU.mult, None,
                 [acck[h], ("rden", h)], [f"osb{b}"])
        fin.append(p.dma(out_d[qt], osb[b][:], reads=[f"osb{b}"]))
    p.final_wait("sp", fin)
    p.emit()
    return nc


def lay_kc(w):
    return np.ascontiguousarray(w.reshape(KC, 128, -1).transpose(1, 0, 2))


def rep128(v):
    return np.ascontiguousarray(np.broadcast_to(np.asarray(v, np.float32).reshape(1, -1), (128, np.asarray(v).size)))


_SL = 2.0 ** (-8.0 * np.arange(1, 9, dtype=np.float64) / 8.0)
SWA_SLOPES = _SL[:4]
NSA_SLOPES = _SL[4:]
OFF = dict(nsa_q=0, kc=512, vc=640, ksl=768, vsl=896, kw=1024, vw=1152, nsa_g=1280, gq=1292, gk=2316, gv=3340,
           gz=4364, ga=5388, gb=5396, sq=5404, sk=5916, sv=6172)


def x_tiles(xb):
    return np.ascontiguousarray(xb.reshape(NT, 128, D))


def band_consts(slopes, nrel):
    j = np.arange(128, dtype=np.float64)
    bias = np.zeros((128, nrel, 2), np.float32)
    for rel in range(nrel):
        for h in range(2):
            bias[:, rel, h] = slopes[h] * (-rel * 128 + j - 127)
    jj, ii = np.meshgrid(np.arange(128), np.arange(128), indexing="ij")
    masks = np.stack([(ii >= jj), (ii < jj)], axis=1).astype(np.float32)
    return bias, np.ascontiguousarray(masks)


def swa_inputs(xb, half, attn_norm, w_in, sinks):
    sl = SWA_SLOPES[2 * half:2 * half + 2]
    cols = np.concatenate([np.arange(OFF["sq"] + 256 * half, OFF["sq"] + 256 * half + 256),
                           np.arange(OFF["sk"] + 128 * half, OFF["sk"] + 128 * half + 128),
                           np.arange(OFF["sv"] + 128 * half, OFF["sv"] + 128 * half + 128)])
    bias, masks = band_consts(sl, 2)
    i = np.arange(128, dtype=np.float64)
    sinkf = np.stack([np.exp(sl[h] * (i - 127)) for h in range(2)], axis=1).astype(np.float32)
    return dict(x=x_tiles(xb), normw=rep128(attn_norm), ident=np.eye(128, dtype=np.float32),
                w=lay_kc(w_in[:, cols]), bias=bias, masks=masks, sinkf=sinkf,
                sinks=rep128(sinks[2 * half:2 * half + 2]))


NCMP = 127
CW = 176


def build_nsa():
    nc = bass.Bass("TRN2", target_bir_lowering=False)
    dt = lambda n, sh, kind="ExternalInput": nc.dram_tensor(n, sh, F32, kind=kind).ap()
    x_d = dt("x", [NT, 128, D])
    nw_d = dt("normw", [128, D])
    id_d = dt("ident", [128, 128])
    wA_d = dt("wA", [128, KC, 512])
    wB_d = dt("wB", [128, KC, 512])
    wC_d = dt("wC", [128, KC, 264])
    w1_d = dt("w1", [2, 128, 32, 256])
    w2_d = dt("w2", [128, 2, 2, 128])
    pos_d = dt("posT", [128, 2, 32, 8])
    biasC_d = dt("biasC", [128, NT, 4])
    biasS_d = dt("biasS", [128, 16, 2])
    maskCmp_d = dt("maskCmp", [128, S])
    expand_d = dt("expand", [32, S])
    bonus_d = dt("bonus", [128, NT, 32])
    ovl_d = dt("ovl", [NCMP, 32])
    mask_d = dt("masks", [128, 2, 128])
    out_d = dt("o", [NT, 128, 256], kind="ExternalOutput")
    p = Prog(nc)
    hT, ident, ps, hn = mixer_prologue(nc, p, x_d, nw_d, id_d)
    w = p.sbuf("w", [128, KC, 512], BF16)
    qT = p.sbuf("qT", [128, 4, S], BF16)
    kcT = p.sbuf("kcT", [128, S], BF16)
    vcT = p.sbuf("vcT", [128, S], BF16)
    kslT = p.sbuf("kslT", [128, S], BF16)
    kwT = p.sbuf("kwT", [128, S], BF16)
    vsl = p.sbuf("vsl", [128, NT, VW], BF16)
    vw = p.sbuf("vw", [128, NT, VW], BF16)
    gsig = p.sbuf("gsig", [128, NT, 8], F32)
    w1 = p.sbuf("w1", [128, 32, 256], BF16)
    w2 = p.sbuf("w2", [128, 2, 2, 128], BF16)
    posT = p.sbuf("posT", [128, 2, 32, 8], BF16)
    hid = p.sbuf("hid", [128, 2, 128], BF16)
    cbias = p.sbuf("cbias", [128, 2], F32)
    kcmpT = p.sbuf("kcmpT", [128, 128], BF16)
    cmpaug = p.sbuf("cmpaug", [128, CW], BF16)
    biasC = p.sbuf("biasC", [128, NT, 4], F32)
    biasS = p.sbuf("biasS", [128, 16, 2], F32)
    maskCmp = p.sbuf("maskCmp", [128, S], F32)
    expand = p.sbuf("expand", [32, S], BF16)
    bonus = p.sbuf("bonus", [128, NT, 32], F32)
    masks = p.sbuf("masks", [128, 2, 128], F32)
    E32 = [p.sbuf(f"E32{b}", [128, 128], F32) for b in range(2)]
    Em = [p.sbuf(f"Em{b}", [128, 128], BF16) for b in range(2)]
    MTs = [p.sbuf(f"MTs{b}", [128, 128], F32) for b in range(2)]
    osb = [p.sbuf(f"osb{b}", [128, 256], F32) for b in range(2)]
    sm = p.sbuf("sm", [128, 8], F32)
    cc = p.sbuf("cc", [128, 8], F32)
    imp = p.sbuf("imp", [128, 32], F32)
    top = p.sbuf("top", [128, 8], F32)
    sel = p.sbuf("sel", [128, 32], F32)
    selT = p.sbuf("selT", [32, 128], BF16)

    p.dma(biasC[:], biasC_d, writes=["biasC"])
    p.dma(biasS[:], biasS_d, writes=["bias"])
    p.dma(maskCmp[:], maskCmp_d, writes=["maskCmp"])
    p.dma(expand[:], expand_d, writes=["expand"], eng="pool")
    p.dma(bonus[:], bonus_d, writes=["bonus"])
    p.dma(masks[:], mask_d, writes=["masks"])
    p.dma(w2[:], w2_d, writes=["w2"], eng="pool")
    p.dma(posT[:], pos_d, writes=["posT"], eng="pool")
    p.op("pool", lambda e: e.memset(vsl[:, :, 128:VW], 1.0), [], ["vsl"])
    p.op("pool", lambda e: e.memset(vw[:, :, 128:VW], 1.0), [], ["vw"])
    p.op("pool", lambda e: e.memset(cmpaug[:, 128:CW], 1.0), [], ["cmpaug"])
    p.dma(cmpaug[0:NCMP, 128:160], ovl_d, reads=[], writes=["cmpaug"], eng="pool")

    cnt = [0]
    p.dma(w[:], wA_d, writes=["w"], eng="pool")
    for h in range(4):
        proj_fm(p, ps, w, "w", h * 128, 128, hT, lambda tg, h=h: qT[:, h, tg * 512:(tg + 1) * 512], lambda tg: "qT", cnt)
    p.dma(w[:], wB_d, writes=["w"], eng="pool")
    proj_fm(p, ps, w, "w", 0, 128, hT, lambda tg: kcT[:, tg * 512:(tg + 1) * 512], lambda tg: "kcT", cnt)
    proj_fm(p, ps, w, "w", 128, 128, hT, lambda tg: vcT[:, tg * 512:(tg + 1) * 512], lambda tg: "vcT", cnt)
    proj_fm(p, ps, w, "w", 256, 128, hT, lambda tg: kslT[:, tg * 512:(tg + 1) * 512], lambda tg: "kslT", cnt)
    for t in range(NT):
        proj_tm(p, ps, w, "w", 384, 128, hT, t, vsl[:, t, 0:128], "vsl", cnt)
    p.dma(w[:, :, 0:264], wC_d, writes=["w"], eng="pool")
    proj_fm(p, ps, w, "w", 0, 128, hT, lambda tg: kwT[:, tg * 512:(tg + 1) * 512], lambda tg: "kwT", cnt)
    for t in range(NT):
        proj_tm(p, ps, w, "w", 128, 128, hT, t, vw[:, t, 0:128], "vw", cnt)
    for t in range(NT):
        proj_tm(p, ps, w, "w", 256, 8, hT, t, gsig[:, t, 0:8], "gsig", cnt)
    p.act(gsig[:], gsig[:], AF.Sigmoid, ["gsig"], ["gsig"])

    for kv in range(2):
        srcT, skey = (kcT, "kcT") if kv == 0 else (vcT, "vcT")
        p.dma(w1[:], w1_d[kv], writes=["w1"], eng="pool")
        for hc in range(2):
            pp, pk = ps[cnt[0] % 2], f"ps{cnt[0] % 2}"
            cnt[0] += 1
            for j in range(32):
                p.mm(pp[:, 0:8], w1[:, j, hc * 128:(hc + 1) * 128], posT[:, kv, j, :], j == 0, j == 31,
                     ["w1", "posT"], [pk])
            p.cp("dve", cbias[:, hc:hc + 1], pp[:, 0:1], [pk], ["cbias"])
        for hc in range(2):
            pp, pk = ps[cnt[0] % 2], f"ps{cnt[0] % 2}"
            cnt[0] += 1
            for j in range(32):
                p.mm(pp[:, 0:NCMP], w1[:, j, hc * 128:(hc + 1) * 128], srcT[:, j:j + 16 * (NCMP - 1) + 1:16], j == 0, j == 31,
                     ["w1", skey], [pk])
            p.act(hid[:, hc, 0:NCMP], pp[:, 0:NCMP], AF.Silu, [pk, "cbias"], ["hid"], bias=cbias[:, hc:hc + 1])
        pp, pk = ps[cnt[0] % 2], f"ps{cnt[0] % 2}"
        cnt[0] += 1
        if kv == 0:
            for hc in range(2):
                p.mm(pp[:, 0:NCMP], w2[:, kv, hc, :], hid[:, hc, 0:NCMP], hc == 0, hc == 1, ["w2", "hid"], [pk])
            p.cp("dve", kcmpT[:, 0:NCMP], pp[:, 0:NCMP], [pk], ["kcmpT"])
        else:
            for hc in range(2):
                p.mm(pp[0:NCMP, 0:128], hid[:, hc, 0:NCMP], w2[:, kv, hc, :], hc == 0, hc == 1, ["w2", "hid"], [pk])
            p.cp("dve", cmpaug[0:NCMP, 0:128], pp[0:NCMP, 0:128], [pk], ["cmpaug"])

    fin = []
    c2 = [0]
    maskC, maskE = masks[:, 0, :], masks[:, 1, :]
    for qt in range(NT):
        ob = osb[qt % 2]
        okey = f"osb{qt % 2}"
        qs = slice(qt * 128, (qt + 1) * 128)
        nn = min(NCMP, 8 * qt + 7)
        p.mm(ps[0][0:nn, 0:512], kcmpT[:, 0:nn], qT[:, 0:4, qs], True, True, ["kcmpT", "qT"], ["ps0"])
        for h in range(4):
            b = c2[0] % 2
            c2[0] += 1
            p.act(E32[b][0:nn, :], ps[0][0:nn, h * 128:(h + 1) * 128], AF.Exp, ["ps0", "biasC"], [f"E32{b}"],
                  bias=biasC[0:nn, qt, h:h + 1], scale=SCALE)
            p.tt("dve", Em[b][0:nn, :], E32[b][0:nn, :], maskCmp[0:nn, qs], ALU.mult, [f"E32{b}", "maskCmp"], [f"Em{b}"])
            ca = ps[2 + h // 2][:, (h % 2) * CW:(h % 2) * CW + 161]
            p.mm(ca, Em[b][0:nn, :], cmpaug[0:nn, 0:161], True, True, [f"Em{b}", "cmpaug"], [f"ps{2 + h // 2}"])
        cacc = [ps[2 + h // 2][:, (h % 2) * CW:(h % 2) * CW + 161] for h in range(4)]
        ck = [f"ps{2 + h // 2}" for h in range(4)]
        for h in range(4):
            p.ts("dve", sm[:, h:h + 1], cacc[h][:, 160:161], 1e-30, None, ALU.max, None, [ck[h]], ["sm"])
        p.op("dve", lambda e: e.reciprocal(sm[:, 4:8], sm[:, 0:4]), ["sm"], ["rs"])
        p.ts("dve", imp[:], cacc[0][:, 128:160], sm[:, 4:5], None, ALU.mult, None, [ck[0], "rs"], ["imp"])
        for h in range(1, 4):
            p.stt("dve", imp[:], cacc[h][:, 128:160], sm[:, 4 + h:5 + h], imp[:], ALU.mult, ALU.add, [ck[h], "rs", "imp"], ["imp"])
        p.tt("dve", imp[:], imp[:], bonus[:, qt, :], ALU.add, ["imp", "bonus"], ["imp"])
        p.op("dve", lambda e: e.max(top[:], imp[:]), ["imp"], ["top"])
        p.ts("dve", sel[:], imp[:], top[:, 7:8], None, ALU.is_ge, None, ["imp", "top"], ["sel"])
        p.tr(ps[1][0:32, 0:128], sel[:, 0:32], ident[:], ["sel", "ident"], ["ps1"])
        p.cp("act", selT[:], ps[1][0:32, 0:128], ["ps1"], ["selT"])
        p.tt("dve", cc[:, 0:2], gsig[:, qt, 0:2], sm[:, 4:6], ALU.mult, ["gsig", "rs"], ["cc"])
        for h in range(2):
            p.ts("dve", ob[:, h * 128:(h + 1) * 128], cacc[h][:, 0:128], cc[:, h:h + 1], None, ALU.mult, None,
                 [ck[h], "cc"], [okey])
        accs = [ps[6][:, 0:129], ps[7][:, 0:129]]
        acck = ["ps6", "ps7"]
        for kt in range(qt + 1):
            rel = qt - kt
            sT, sTk = ps[4 + kt % 2], f"ps{4 + kt % 2}"
            p.mm(sT[:, 0:256], kslT[:, kt * 128:(kt + 1) * 128], qT[:, 0:2, qs], True, True, ["kslT", "qT"], [sTk])
            p.mm(ps[1][:, 128:256], expand[0:32, kt * 128:(kt + 1) * 128], selT[0:32, :], True, True,
                 ["expand", "selT"], ["ps1"])
            mb = kt % 2
            if rel == 0:
                p.tt("dve", MTs[mb][:], ps[1][:, 128:256], maskC, ALU.mult, ["ps1", "masks"], [f"MTs{mb}"])
            else:
                p.cp("act", MTs[mb][:], ps[1][:, 128:256], ["ps1"], [f"MTs{mb}"])
            for h in range(2):
                b = c2[0] % 2
                c2[0] += 1
                p.act(E32[b][:], sT[:, h * 128:(h + 1) * 128], AF.Exp, [sTk, "bias"], [f"E32{b}"],
                      bias=biasS[:, rel, h:h + 1], scale=SCALE)
                p.tt("pool" if h == 0 else "dve", Em[b][:], E32[b][:], MTs[mb][:], ALU.mult,
                     [f"E32{b}", f"MTs{mb}"], [f"Em{b}"])
                p.mm(accs[h], Em[b][:], vsl[:, kt, 0:129], kt == 0, kt == qt, [f"Em{b}", "vsl"], [acck[h]])

        def combine(br):
            for h in range(2):
                p.op("dve", lambda e, h=h: e.reciprocal(cc[:, 2 + h:3 + h], accs[h][:, 128:129]), [acck[h]], ["cc2"])
            p.tt("dve", cc[:, 4:6], gsig[:, qt, 2 * br:2 * br + 2], cc[:, 2:4], ALU.mult, ["gsig", "cc2"], ["cc3"])
            for h in range(2):
                os_ = ob[:, h * 128:(h + 1) * 128]
                p.stt("dve", os_, accs[h][:, 0:128], cc[:, 4 + h:5 + h], os_, ALU.mult, ALU.add, [acck[h], "cc3", okey], [okey])

        combine(1)
        emit_band(p, qt, 4, qT, "qT", kwT, "kwT", vw, "vw", biasS, maskC, maskE, ps[4], "ps4", accs, acck, E32, Em, c2)
        combine(2)
        fin.append(p.dma(out_d[qt], ob[:], reads=[okey]))
    p.final_wait("sp", fin)
    p.emit()
    return nc


def nsa_inputs(xb, half, attn_norm, w_in, pos_k, pos_v, w1_k, w2_k, w1_v, w2_v):
    mine = [2 * half, 2 * half + 1]
    other = [2 * (1 - half), 2 * (1 - half) + 1]
    hq = mine + other
    colsA = np.concatenate([np.arange(OFF["nsa_q"] + h * 128, OFF["nsa_q"] + (h + 1) * 128) for h in hq])
    colsB = np.arange(OFF["kc"], OFF["kc"] + 512)
    gcols = [OFF["nsa_g"] + br * 4 + h for br in range(3) for h in mine]
    colsC = np.concatenate([np.arange(OFF["kw"], OFF["kw"] + 256), np.array(gcols + gcols[:2])])
    sl4 = [NSA_SLOPES[h] for h in hq]
    n = np.arange(128, dtype=np.float64)
    biasC = np.zeros((128, NT, 4), np.float32)
    for qt in range(NT):
        for h in range(4):
            biasC[:, qt, h] = sl4[h] * (16 * n + 31 - (128 * qt + 127))
    biasS, masks = band_consts(sl4[:2], 16)
    t = np.arange(S)
    maskCmp = ((16 * np.arange(128)[:, None] + 31) <= t[None, :]).astype(np.float32)
    maskCmp[127] = 0
    expand = (t[None, :] // 64 == np.arange(32)[:, None]).astype(np.float32)
    cur = t // 64
    blk = np.arange(32)
    valid = blk[None] <= cur[:, None]
    forced = (blk[None] == 0) | (blk[None] == cur[:, None]) | (blk[None] == cur[:, None] - 1)
    bonus = np.where(valid, np.where(forced, 1000.0, 0.0), -1.0).astype(np.float32)
    bonus = np.ascontiguousarray(bonus.reshape(NT, 128, 32).transpose(1, 0, 2))
    c_start = np.arange(NCMP) * 16
    s_start = np.arange(32) * 64
    ovl = np.clip(np.minimum(c_start[:, None] + 32, s_start[None] + 64) - np.maximum(c_start[:, None], s_start[None]), 0, None)
    ovl = (ovl / 32.0).astype(np.float32)
    lay_w1 = lambda w1: np.ascontiguousarray(w1.reshape(32, 128, 256).transpose(1, 0, 2))
    lay_w2 = lambda w2: w2.reshape(2, 128, 128).transpose(1, 0, 2)
    return dict(x=x_tiles(xb), normw=rep128(attn_norm), ident=np.eye(128, dtype=np.float32),
                wA=lay_kc(w_in[:, colsA]), wB=lay_kc(w_in[:, colsB]), wC=lay_kc(w_in[:, colsC]),
                w1=np.stack([lay_w1(w1_k), lay_w1(w1_v)]),
                w2=np.ascontiguousarray(np.stack([lay_w2(w2_k), lay_w2(w2_v)], axis=1)),
                posT=np.ascontiguousarray(np.broadcast_to(np.stack([pos_k.T, pos_v.T], axis=1)[..., None], (128, 2, 32, 8))),
                biasC=biasC, biasS=biasS, maskCmp=maskCmp, expand=expand, bonus=bonus, ovl=ovl, masks=masks)


CH = 64
NCH = S // CH


def build_gdn(stage=0, nheads=4, nchunks=NCH, limit=None):
    nc = bass.Bass("TRN2", target_bir_lowering=False)
    dt = lambda n, sh, kind="ExternalInput": nc.dram_tensor(n, sh, F32, kind=kind).ap()
    x_d = dt("x", [NT, 128, D])
    nw_d = dt("normw", [128, D])
    id_d = dt("ident", [128, 128])
    wh_d = dt("wh", [4, 128, KC, 512])
    wab_d = dt("wab", [128, KC, 8])
    conv_d = dt("convw", [128, 4, 3, 4])
    alog_d = dt("alog", [CH, NCH, 4])
    dtb_d = dt("dtb", [CH, NCH, 4])
    gnw_d = dt("gnw", [CH, 128])
    tri_d = dt("tri", [CH, CH])
    msl_d = dt("msl", [CH, CH])
    out_d = dt("o", [4, NCH, CH, 128], kind="ExternalOutput")
    dbg_d = dt("dbg", [128, 8, S], kind="ExternalOutput") if stage else None
    p = Prog(nc)
    hT, ident, ps, hn = mixer_prologue(nc, p, x_d, nw_d, id_d)
    sq, rn = hn, None
    wbuf = p.sbuf("wbuf", [128, KC, 512], BF16)
    wab = p.sbuf("wab", [128, KC, 8], BF16)
    pre = p.sbuf("pre", [128, S], F32)
    rnb = p.sbuf("rnb", [128, S], F32)
    yq = p.sbuf("yq", [128, S], F32)
    yk = p.sbuf("yk", [128, S], F32)
    yv = p.sbuf("yv", [128, S], F32)
    zs = p.sbuf("zs", [CH, NCH, 128], F32)
    convw = p.sbuf("convw", [128, 4, 3, 4], F32)
    gnw = p.sbuf("gnw", [CH, 128], F32)
    tri = p.sbuf("tri", [CH, CH], F32)
    msl = p.sbuf("msl", [CH, CH], F32)
    ones = p.sbuf("ones", [128, 128], F32)
    ab = p.sbuf("ab", [CH, NCH, 8], F32)
    alog = p.sbuf("alog", [CH, NCH, 4], F32)
    dtb = p.sbuf("dtb", [CH, NCH, 4], F32)
    g = p.sbuf("g", [CH, NCH, 4], F32)
    beta = p.sbuf("beta", [CH, NCH, 4], F32)
    nbeta = p.sbuf("nbeta", [CH, NCH, 4], F32)
    gc = p.sbuf("gc", [CH, NCH, 4], F32)
    bsc = p.sbuf("bsc", [CH, NCH, 4], F32)
    kdsc = p.sbuf("kdsc", [CH, NCH, 4], F32)
    egl = p.sbuf("egl", [128, NCH, 4], F32)
    tmp4 = p.sbuf("tmp4", [CH, NCH, 4], F32)
    St = p.sbuf("St", [128, 128], F32)
    nb = 2
    mk = lambda n, sh: [p.sbuf(f"{n}{i}", sh, F32) for i in range(nb)]
    Wk, Vb, kdec, gB, dmn, DTm, Dm = (mk(n, sh) for n, sh in [("Wk", [CH, 128]), ("Vb", [CH, 128]), ("kdec", [CH, 128]),
                                                             ("gB", [CH, 128]), ("dmn", [CH, CH]), ("DTm", [CH, CH]), ("Dm", [CH, CH])])
    Xb, Yb, Rb = mk("Xb", [CH, CH]), mk("Yb", [CH, CH]), mk("Rb", [CH, CH])
    attnT, u_sb, wT, qdT, egB, vnew, osm = (mk(n, sh) for n, sh in [("attnT", [CH, CH]), ("u", [CH, 128]), ("wT", [128, CH]),
                                                                    ("qdT", [128, CH]), ("egB", [128, CH]), ("vnew", [CH, 128]), ("osm", [CH, 128])])
    ssn = p.sbuf("ssn", [CH, 4], F32)
    junk = p.sbuf("junk", [CH, 128], F32)

    for tgt, src, k in [(convw, conv_d, "convw"), (gnw, gnw_d, "gnw"), (tri, tri_d, "tri"), (msl, msl_d, "msl"),
                        (alog, alog_d, "alog"), (dtb, dtb_d, "dtb")]:
        p.dma(tgt[:], src, writes=[k])
    p.dma(wab[:], wab_d, writes=["wab"], eng="pool")
    p.op("pool", lambda e: e.memset(ones[:], 1.0), [], ["ones"])

    cnt = [0]
    rr = [0]

    def bank():
        i = 2 + rr[0] % 6
        rr[0] += 1
        return ps[i], f"ps{i}"

    for c in range(NCH):
        proj_tm(p, ps, wab, "wab", 0, 8, hT, None, ab[:, c, :], "ab", cnt, ntok=CH, tok0=c * CH)
    p.act(beta[:], ab[:, :, 4:8], AF.Sigmoid, ["ab"], ["beta"])
    p.ts("dve", nbeta[:], beta[:], -1.0, None, ALU.mult, None, ["beta"], ["nbeta"])
    p.tt("dve", tmp4[:], ab[:, :, 0:4], dtb[:], ALU.add, ["ab", "dtb"], ["tmp4"])
    p.act(tmp4[:], tmp4[:], AF.Exp, ["tmp4"], ["tmp4"])
    p.act(tmp4[:], tmp4[:], AF.Ln, ["tmp4"], ["tmp4"], bias=1.0)
    p.act(alog[:], alog[:], AF.Exp, ["alog"], ["alog"])
    p.stt("dve", g[:], tmp4[:], -1.0, alog[:], ALU.mult, ALU.mult, ["tmp4", "alog"], ["g"])
    g2 = g[:].rearrange("p c h -> p (c h)")
    pp, pk = bank()
    p.mm(pp[0:CH, 0:128], tri[:], g2, True, True, ["tri", "g"], [pk])
    p.cp("dve", gc[:].rearrange("p c h -> p (c h)"), pp[0:CH, 0:128], [pk], ["gc"])
    pp, pk = bank()
    p.mm(pp[:, 0:128], ones[0:CH, :], g2, True, True, ["ones", "g"], [pk])
    p.act(egl[:].rearrange("p c h -> p (c h)"), pp[:, 0:128], AF.Exp, [pk], ["egl"])
    p.tt("dve", kdsc[:].rearrange("p c h -> p (c h)"), pp[0:CH, 0:128], gc[:].rearrange("p c h -> p (c h)"),
         ALU.subtract, [pk, "gc"], ["kdsc"])
    p.act(kdsc[:], kdsc[:], AF.Exp, ["kdsc"], ["kdsc"])
    p.act(tmp4[:], gc[:], AF.Exp, ["gc", "tmp4"], ["tmp4"])
    p.tt("dve", bsc[:], tmp4[:], beta[:], ALU.mult, ["tmp4", "beta"], ["bsc"])

    fin = []
    if stage == 1:
        for i, (t_, k_, np_) in enumerate([(gc, "gc", CH), (egl, "egl", 128), (kdsc, "kdsc", CH), (bsc, "bsc", CH), (g, "g", CH), (beta, "beta", CH)]):
            fin.append(p.dma(dbg_d[0:np_, i, 0:128], t_[:].rearrange("p c h -> p (c h)"), reads=[k_]))
        p.final_wait("sp", fin)
        p.emit()
        return nc
    for h in range(nheads):
        p.dma(wbuf[:], wh_d[h], writes=["wbuf"], eng="pool")
        for ti, (y, yk_) in enumerate([(yq, "yq"), (yk, "yk"), (yv, "yv")]):
            proj_fm(p, ps, wbuf, "wbuf", ti * 128, 128, hT, lambda tg: pre[:, tg * 512:(tg + 1) * 512], lambda tg: "pre", cnt)
            eng = "dve"
            cw = lambda tap: convw[:, h, ti, tap:tap + 1]
            p.ts(eng, y[:], pre[:], cw(3), None, ALU.mult, None, ["pre", "convw"], [yk_])
            for sh in (1, 2, 3):
                p.stt(eng, y[:, sh:S], pre[:, 0:S - sh], cw(3 - sh), y[:, sh:S], ALU.mult, ALU.add,
                      ["pre", "convw", yk_], [yk_])
            p.act(y[:], y[:], AF.Silu, [yk_], [yk_])
        for c in range(NCH):
            proj_tm(p, ps, wbuf, "wbuf", 384, 128, hT, None, zs[:, c, :], "zs", cnt, ntok=CH, tok0=c * CH)
        p.act(zs[:], zs[:], AF.Silu, ["zs"], ["zs"])
        for y, yk_, sc in [(yq, "yq", SCALE), (yk, "yk", 1.0)]:
            p.tt("pool", sq[:], y[:], y[:], ALU.mult, [yk_], ["hn"])
            for tg in range(4):
                pp, pk = ps[cnt[0] % 2], f"ps{cnt[0] % 2}"
                cnt[0] += 1
                p.mm(pp[:], ones[:], sq[:, tg * 512:(tg + 1) * 512], True, True, ["ones", "hn"], [pk])
                p.act(rnb[:, tg * 512:(tg + 1) * 512], pp[:], AF.Ln, [pk], ["rnb"], bias=EPS)
            p.act(rnb[:], rnb[:], AF.Exp, ["rnb"], ["rnb"], scale=-0.5)
            p.stt("dve", y[:], y[:], sc, rnb[:], ALU.mult, ALU.mult, [yk_, "rnb"], [yk_])
        if stage == 2:
            for i, (t_, k_) in enumerate([(yq, "yq"), (yk, "yk"), (yv, "yv")]):
                fin.append(p.dma(dbg_d[:, i, :], t_[:], reads=[k_]))
            fin.append(p.dma(dbg_d[0:CH, 3, :], zs[:, 0:16, :].rearrange("p c d -> p (c d)"), reads=["zs"]))
            p.final_wait("sp", fin)
            p.emit()
            return nc
        print("ops before chunk loop", p.n_ops)
        p.op("pool", lambda e: e.memset(St[:], 0.0), [], ["St"])
        for c in range(nchunks):
            b = c % nb
            cs = slice(c * CH, (c + 1) * CH)
            col = lambda t: t[:, c, h:h + 1]
            K = lambda n: f"{n}{b}"
            pp, pk = bank()
            p.tr(pp[0:CH, 0:128], yk[:, cs], ident[:], ["yk", "ident"], [pk])
            p.ts("dve", Wk[b][:], pp[0:CH, 0:128], col(bsc), None, ALU.mult, None, [pk, "bsc"], [K("Wk")])
            p.ts("act" if False else "dve", kdec[b][:], pp[0:CH, 0:128], col(kdsc), None, ALU.mult, None, [pk, "kdsc"], [K("kdec")])
            pp, pk = bank()
            p.tr(pp[0:CH, 0:128], yv[:, cs], ident[:], ["yv", "ident"], [pk])
            p.ts("dve", Vb[b][:], pp[0:CH, 0:128], col(beta), None, ALU.mult, None, [pk, "beta"], [K("Vb")])
            p.ts("pool", gB[b][:], ones[0:CH, :], col(g), None, ALU.mult, None, ["ones", "g"], [K("gB")])
            pg, pgk = bank()
            p.mm(pg[:, 0:CH], gB[b][:], tri[:], True, True, [K("gB"), "tri"], [pgk])
            p.ts("dve", dmn[b][:], pg[0:CH, 0:CH], col(gc), 0.0, ALU.subtract, ALU.min, [pgk, "gc"], [K("dmn")])
            p.act(DTm[b][:], dmn[b][:], AF.Exp, [K("dmn")], [K("DTm")])
            p.tt("pool", DTm[b][:], DTm[b][:], tri[:], ALU.mult, [K("DTm"), "tri"], [K("DTm")])
            p.ts("dve", dmn[b][:], pg[0:CH, 0:CH], col(gc), 0.0, ALU.subtract, ALU.max, [pgk, "gc"], [K("dmn")])
            p.act(Dm[b][:], dmn[b][:], AF.Exp, [K("dmn")], [K("Dm")], scale=-1.0)
            p.tt("pool", Dm[b][:], Dm[b][:], msl[:], ALU.mult, [K("Dm"), "msl"], [K("Dm")])
            p.act(egB[b][:], pg[:, 0:CH], AF.Exp, [pgk], [K("egB")])
            p.tt("pool", qdT[b][:], yq[:, cs], egB[b][:], ALU.mult, ["yq", K("egB")], [K("qdT")])
            pp, pk = bank()
            p.mm(pp[0:CH, 0:CH], yk[:, cs], yk[:, cs], True, True, ["yk"], [pk])
            xi = 0
            p.stt("dve", Xb[xi][:], pp[0:CH, 0:CH], col(nbeta), Dm[b][:], ALU.mult, ALU.mult, [pk, "nbeta", K("Dm")], ["Xb0"])
            pp, pk = bank()
            p.mm(pp[0:CH, 0:CH], yk[:, cs], yq[:, cs], True, True, ["yk", "yq"], [pk])
            p.tt("dve", attnT[b][:], pp[0:CH, 0:CH], DTm[b][:], ALU.mult, [pk, K("DTm")], [K("attnT")])
            pp, pk = bank()
            p.tr(pp[0:CH, 0:CH], Xb[0][:], ident[0:CH, 0:CH], ["Xb0", "ident"], [pk])
            p.cp("act", Yb[0][:], pp[0:CH, 0:CH], [pk], ["Yb0"])
            p.tt("dve", Rb[0][:], pp[0:CH, 0:CH], ident[0:CH, 0:CH], ALU.add, [pk, "ident"], ["Rb0"])
            xi = yi = ri = 0
            for lv in range(5):
                xn, yn, rn_ = 1 - xi, 1 - yi, 1 - ri
                pp, pk = bank()
                p.mm(pp[0:CH, 0:CH], Yb[yi][:], Xb[xi][:], True, True, [f"Yb{yi}", f"Xb{xi}"], [pk])
                if lv < 4:
                    pp2, pk2 = bank()
                    p.mm(pp2[0:CH, 0:CH], Xb[xi][:], Yb[yi][:], True, True, [f"Yb{yi}", f"Xb{xi}"], [pk2])
                p.cp("act", Xb[xn][:], pp[0:CH, 0:CH], [pk], [f"Xb{xn}"])
                if lv < 4:
                    p.cp("dve", Yb[yn][:], pp2[0:CH, 0:CH], [pk2], [f"Yb{yn}"])
                pp3, pk3 = bank()
                p.mm(pp3[0:CH, 0:CH], Xb[xn][:], Rb[ri][:], True, True, [f"Xb{xn}", f"Rb{ri}"], [pk3])
                p.tt("dve", Rb[rn_][:], pp3[0:CH, 0:CH], Rb[ri][:], ALU.add, [pk3, f"Rb{ri}"], [f"Rb{rn_}"])
                xi, yi, ri = xn, yn, rn_
            AinvT, ak = Rb[ri], f"Rb{ri}"
            pp, pk = bank()
            p.mm(pp[0:CH, 0:128], AinvT[:], Vb[b][:], True, True, [ak, K("Vb")], [pk])
            p.cp("act", u_sb[b][:], pp[0:CH, 0:128], [pk], [K("u")])
            pp, pk = bank()
            p.mm(pp[:, 0:CH], Wk[b][:], AinvT[:], True, True, [ak, K("Wk")], [pk])
            p.cp("dve", wT[b][:], pp[:, 0:CH], [pk], [K("wT")])
            pp, pk = bank()
            p.mm(pp[0:CH, 0:128], wT[b][:], St[:], True, True, [K("wT"), "St"], [pk])
            p.tt("dve", vnew[b][:], u_sb[b][:], pp[0:CH, 0:128], ALU.subtract, [K("u"), pk], [K("vnew")])
            po, pok = bank()
            p.mm(po[0:CH, 0:128], qdT[b][:], St[:], True, False, [K("qdT"), "St"], [pok])
            p.mm(po[0:CH, 0:128], attnT[b][:], vnew[b][:], False, True, [K("attnT"), K("vnew")], [pok])
            pS, pSk = bank()
            p.mm(pS[:, 0:128], kdec[b][:], vnew[b][:], True, True, [K("kdec"), K("vnew")], [pSk])
            p.stt("dve", St[:], St[:], egl[:, c, h:h + 1], pS[:, 0:128], ALU.mult, ALU.add, ["St", "egl", pSk], ["St"])
            p.act(junk[:], po[0:CH, 0:128], AF.Square, [pok], ["junk", "ssn0"], accum_out=ssn[:, 0:1])
            p.act(ssn[:, 1:2], ssn[:, 0:1], AF.Ln, ["ssn0"], ["ssn1"], bias=EPS, scale=1.0 / 128)
            p.act(ssn[:, 1:2], ssn[:, 1:2], AF.Exp, ["ssn1"], ["ssn1"], scale=-0.5)
            p.stt("dve", osm[b][:], po[0:CH, 0:128], ssn[:, 1:2], gnw[:], ALU.mult, ALU.mult, [pok, "ssn1", "gnw"], [K("osm")])
            p.tt("pool", osm[b][:], osm[b][:], zs[:, c, :], ALU.mult, [K("osm"), "zs"], [K("osm")])
            fin.append(p.dma(out_d[h, c], osm[b][:], reads=[K("osm")]))
            print("ops after chunk", c, p.n_ops)
            if c == 0 and limit is not None:
                p.limit = abs(limit)
                p.serial = limit < 0
    p.final_wait("sp", fin)
    p.emit()
    return nc


def gdn_inputs(xb, half, attn_norm, w_in, conv_w, a_log, dt_bias, norm_w):
    heads = [4 * half + i for i in range(4)]
    wh = []
    for gh in heads:
        cols = np.concatenate([np.arange(OFF[k] + gh * 128, OFF[k] + (gh + 1) * 128) for k in ("gq", "gk", "gv", "gz")])
        wh.append(lay_kc(w_in[:, cols]))
    cab = np.array([OFF["ga"] + gh for gh in heads] + [OFF["gb"] + gh for gh in heads])
    convw = np.zeros((128, 4, 3, 4), np.float32)
    for i, gh in enumerate(heads):
        for ti in range(3):
            convw[:, i, ti, :] = conv_w[:, ti * 1024 + gh * 128: ti * 1024 + (gh + 1) * 128].T
    rep = lambda v: np.ascontiguousarray(np.broadcast_to(np.asarray(v, np.float32)[heads].reshape(1, 1, 4), (CH, NCH, 4)))
    jj, ii = np.meshgrid(np.arange(CH), np.arange(CH), indexing="ij")
    return dict(x=x_tiles(xb), normw=rep128(attn_norm), ident=np.eye(128, dtype=np.float32),
                wh=np.stack(wh), wab=lay_kc(w_in[:, cab]), convw=convw, alog=rep(a_log), dtb=rep(dt_bias),
                gnw=np.ascontiguousarray(np.broadcast_to(norm_w.reshape(1, 128), (CH, 128))),
                tri=(jj <= ii).astype(np.float32), msl=(jj > ii).astype(np.float32))


def lay_fd(w):
    return np.ascontiguousarray(w.reshape(DFF // 128, 128, D).transpose(1, 0, 2))


_W_CACHE = {}


def ffn_weights(layer, inputs):
    key = layer
    if key in _W_CACHE:
        return _W_CACHE[key]
    d = dict(wout=lay_kc(inputs["w_out"][layer]), normw=rep128(inputs["ffn_norm"][layer]),
             ident=np.eye(128, dtype=np.float32))
    if layer % 2 == 0:
        i = layer // 2
        d.update(wg=lay_kc(inputs["dense_w_gate"][i])[None], wu=lay_kc(inputs["dense_w_up"][i])[None],
                 wd=lay_fd(inputs["dense_w_down"][i])[None])
    else:
        i = layer // 2
        d.update(wg=np.stack([lay_kc(inputs["moe_w_gate"][i][e]) for e in range(NEXP)]),
                 wu=np.stack([lay_kc(inputs["moe_w_up"][i][e]) for e in range(NEXP)]),
                 wd=np.stack([lay_fd(inputs["moe_w_down"][i][e]) for e in range(NEXP)]),
                 router=lay_kc(inputs["moe_router"][i]))
    if layer == 1:
        d["fnormw"] = rep128(inputs["final_norm"])
    _W_CACHE[key] = d
    return d


def ffn_inputs(xrows, orows, layer, inputs):
    d = dict(ffn_weights(layer, inputs))
    d["xin"] = np.ascontiguousarray(xrows.reshape(NT_F, 128, D))
    d["oT"] = np.ascontiguousarray(orows.T.reshape(KC, 128, NT_F * 128).transpose(1, 0, 2))
    return d


_NC_CACHE = {}


def _get_nc(name, fn):
    if name not in _NC_CACHE:
        _NC_CACHE[name] = fn()
    return _NC_CACHE[name]


def kernel(**inputs):
    inputs = {k: np.asarray(v) for k, v in inputs.items()}
    _W_CACHE.clear()
    x = np.ascontiguousarray(inputs["x"], dtype=np.float32)
    cores = list(range(8))
    for layer in range(2):
        g = lambda n: inputs[n][layer]
        maps = [swa_inputs(x[c // 2], c % 2, g("attn_norm"), g("w_in"), g("swa_sinks")) for c in cores]
        r_swa = run_bass_kernel_spmd(_get_nc("swa", build_swa), maps, core_ids=cores).results
        maps = [nsa_inputs(x[c // 2], c % 2, g("attn_norm"), g("w_in"), g("cmp_pos_k"), g("cmp_pos_v"),
                           g("cmp_w1_k"), g("cmp_w2_k"), g("cmp_w1_v"), g("cmp_w2_v")) for c in cores]
        r_nsa = run_bass_kernel_spmd(_get_nc("nsa", build_nsa), maps, core_ids=cores).results
        maps = [gdn_inputs(x[c // 2], c % 2, g("attn_norm"), g("w_in"), g("gdn_conv_w"), g("gdn_a_log"),
                           g("gdn_dt_bias"), g("gdn_norm_w")) for c in cores]
        r_gdn = run_bass_kernel_spmd(_get_nc("gdn", build_gdn), maps, core_ids=cores).results
        o = np.empty((NB, S, D), np.float32)
        for c in cores:
            b, half = c // 2, c % 2
            o[b, :, half * 256:(half + 1) * 256] = r_nsa[c]["o"].reshape(S, 256)
            o[b, :, 512 + half * 512:512 + (half + 1) * 512] = (
                r_gdn[c]["o"].reshape(4, S, 128).transpose(1, 0, 2).reshape(S, 512))
            o[b, :, 1536 + half * 256:1536 + (half + 1) * 256] = r_swa[c]["o"].reshape(S, 256)
        n_exp = 1 if layer % 2 == 0 else NEXP
        final = layer == 1
        maps = []
        for c in cores:
            b, r0 = c // 2, (c % 2) * 1024
            maps.append(ffn_inputs(x[b, r0:r0 + 1024], o[b, r0:r0 + 1024], layer, inputs))
        r_ffn = run_bass_kernel_spmd(_get_nc(("ffn", n_exp, final), lambda: build_ffn(n_exp, final)), maps,
                                     core_ids=cores).results
        xn = np.empty_like(x)
        for c in cores:
            b, r0 = c // 2, (c % 2) * 1024
            xn[b, r0:r0 + 1024] = r_ffn[c]["xout"].reshape(1024, D)
        x = xn
    _W_CACHE.clear()
    return x
```
